# Optimizing a Trainium2 kernel written in Bass

```python
import math
import jax, jax.numpy as jnp
from jax import lax
import numpy as np

D_MODEL = 2048
BATCH = 4
SEQ = 4096
DEPTH = 1

HG_HEADS = 8
HG_KDIM = 128
HG_VDIM = 128
HG_WIDTH = HG_HEADS * HG_VDIM
HG_FDIM = HG_HEADS * HG_KDIM
HG_CHUNK = 64

NSA_HEADS = 16
NSA_GROUPS = 4
NSA_HPG = NSA_HEADS // NSA_GROUPS
NSA_DH = 64
NSA_WIDTH = NSA_HEADS * NSA_DH
NSA_KV = NSA_GROUPS * NSA_DH
CMP_LEN = 32
CMP_STRIDE = 16
CMP_HIDDEN = 256
SLC_LEN = 64
SLC_TOPK = 16
WIN = 512
NSA_QBLOCK = 64

MIX_WIDTH = HG_WIDTH + NSA_WIDTH

REL_BUCKETS = 32
REL_MAX_DIST = 128

N_EXPERT_GROUPS = 8
EXPERTS_PER_GROUP = 8
N_EXPERTS = N_EXPERT_GROUPS * EXPERTS_PER_GROUP
EXPERT_TOPK = 2
EXPERT_DFF = 1024
MOE_BLOCK = 128

RMS_EPS = 1e-6
NEG_INF = -1e30
BIG = 1e9

IN_SIZES = (HG_FDIM, HG_FDIM, HG_WIDTH, HG_WIDTH,
            NSA_WIDTH, NSA_KV, NSA_KV, NSA_KV, NSA_KV, NSA_KV, NSA_KV, 3 * NSA_HEADS)
IN_COLS = 2 * HG_FDIM + 2 * HG_WIDTH + NSA_WIDTH + 6 * NSA_KV + 3 * NSA_HEADS

kernel_name = "hybrid_hgrn2_nsa_hmoe_block"


def rmsnorm(x, g):
    xf = x.astype(jnp.float32)
    y = xf * lax.rsqrt(jnp.mean(xf * xf, axis=-1, keepdims=True) + RMS_EPS)
    return (y * g.astype(jnp.float32)).astype(x.dtype)


def masked_softmax(logits, mask):
    logits = jnp.where(mask, logits.astype(jnp.float32), NEG_INF)
    p = jax.nn.softmax(logits, axis=-1)
    return p * mask.astype(p.dtype)


def rel_bucket(dist):
    n = jnp.maximum(dist, 0)
    max_exact = REL_BUCKETS // 2
    nf = jnp.maximum(n, 1).astype(jnp.float32)
    large = max_exact + (jnp.log(nf / max_exact) / math.log(REL_MAX_DIST / max_exact)
                         * (REL_BUCKETS - max_exact)).astype(jnp.int32)
    large = jnp.minimum(large, REL_BUCKETS - 1)
    return jnp.where(n < max_exact, n, large)


def hgrn2_mixer(q, f_logit, i_in, g_out, lower_bound, norm_g):
    B, S = q.shape[0], q.shape[1]
    nc = S // HG_CHUNK
    f32 = jnp.float32
    shp_k = (B, S, HG_HEADS, HG_KDIM)
    shp_v = (B, S, HG_HEADS, HG_VDIM)
    q = jax.nn.silu(q.reshape(shp_k).astype(f32))
    lb = lower_bound.reshape(HG_HEADS, HG_KDIM)
    f = lb + (1.0 - lb) * jax.nn.sigmoid(f_logit.reshape(shp_k).astype(f32))
    log_f = jnp.log(f)
    k = 1.0 - f
    v = i_in.reshape(shp_v).astype(f32)

    def chunks(t):
        return t.reshape(B, nc, HG_CHUNK, HG_HEADS, t.shape[-1]).transpose(1, 0, 3, 2, 4)

    causal = jnp.tril(jnp.ones((HG_CHUNK, HG_CHUNK), dtype=bool))[:, :, None]

    def step(state, inp):
        qc, kc, vc, lfc = inp
        b = jnp.cumsum(lfc, axis=2)
        decay = jnp.exp(jnp.where(causal, b[:, :, :, None, :] - b[:, :, None, :, :], NEG_INF))
        scores = jnp.einsum('bhtd,bhsd,bhtsd->bhts', qc, kc, decay)
        o = (jnp.einsum('bhts,bhsv->bhtv', scores, vc)
             + jnp.einsum('bhtd,bhdv->bhtv', qc * jnp.exp(b), state))
        b_last = b[:, :, -1:, :]
        state = (jnp.exp(b_last[:, :, 0, :, None]) * state
                 + jnp.einsum('bhsd,bhsv->bhdv', kc * jnp.exp(b_last - b), vc))
        return state, o

    s0 = jnp.zeros((B, HG_HEADS, HG_KDIM, HG_VDIM), f32)
    _, o = lax.scan(step, s0, (chunks(q), chunks(k), chunks(v), chunks(log_f)))
    o = o.transpose(1, 0, 3, 2, 4).reshape(shp_v)
    o = o * lax.rsqrt(jnp.mean(o * o, axis=-1, keepdims=True) + RMS_EPS) * norm_g.astype(f32)
    o = o * jax.nn.silu(g_out.reshape(shp_v).astype(f32))
    return o.reshape(B, S, HG_WIDTH)


def nsa_mixer(q, k_cmp, v_cmp, k_slc, v_slc, k_win, v_win, gate_logits,
              cmp_pe_k, cmp_w1_k, cmp_w2_k, cmp_pe_v, cmp_w1_v, cmp_w2_v, rel_bias):
    B, S = q.shape[0], q.shape[1]
    f32 = jnp.float32
    C = NSA_QBLOCK
    nq = S // C
    n_cmp = (S - CMP_LEN) // CMP_STRIDE + 1
    n_slc = S // SLC_LEN
    top_k = min(SLC_TOPK, n_slc)
    L = top_k * SLC_LEN

    def kv(t):
        return t.reshape(B, S, NSA_GROUPS, NSA_DH)

    cmp_idx = np.arange(n_cmp)[:, None] * CMP_STRIDE + np.arange(CMP_LEN)[None, :]
    cmp_end = jnp.asarray(cmp_idx[:, -1], jnp.int32)

    def compress(t, pe, w1, w2):
        blk = kv(t)[:, cmp_idx] + pe[None, None, :, None, :]
        blk = blk.transpose(0, 3, 1, 2, 4).reshape(B, NSA_GROUPS, n_cmp, CMP_LEN * NSA_DH)
        return jax.nn.silu(blk @ w1) @ w2

    kc = compress(k_cmp, cmp_pe_k, cmp_w1_k, cmp_w2_k)
    vc = compress(v_cmp, cmp_pe_v, cmp_w1_v, cmp_w2_v)

    ci = np.arange(n_cmp)[:, None] * CMP_STRIDE
    sj = np.arange(n_slc)[None, :] * SLC_LEN
    overlap = jnp.asarray(((ci <= sj + SLC_LEN - 1) & (ci + CMP_LEN - 1 >= sj)).astype(np.float32))

    def blocks(t):
        return kv(t).reshape(B, n_slc, SLC_LEN, NSA_GROUPS, NSA_DH).transpose(0, 3, 1, 2, 4)

    ks_blk = blocks(k_slc)
    vs_blk = blocks(v_slc)

    def pad_win(t):
        return jnp.pad(kv(t), ((0, 0), (WIN, 0), (0, 0), (0, 0))).transpose(0, 2, 1, 3)

    kw_pad = pad_win(k_win)
    vw_pad = pad_win(v_win)

    table = rel_bias.T.reshape(NSA_GROUPS, NSA_HPG, REL_BUCKETS)
    ii = np.arange(C)[:, None]
    jj = np.arange(WIN + C)[None, :]
    dist_w = WIN + ii - jj
    band = jnp.asarray((dist_w >= 0) & (dist_w < WIN))
    bias_w = table[:, :, rel_bucket(jnp.asarray(dist_w, jnp.int32))]
    g_idx = jnp.arange(NSA_GROUPS)[None, :, None, None, None]
    h_idx = jnp.arange(NSA_HPG)[None, None, :, None, None]
    gather = jax.vmap(jax.vmap(lambda blk, ix: blk[ix]))

    qb = (q.reshape(B, nq, C, NSA_GROUPS, NSA_HPG, NSA_DH) * NSA_DH ** -0.5).transpose(1, 0, 3, 4, 2, 5)
    gb = jax.nn.sigmoid(gate_logits.astype(f32)).reshape(
        B, nq, C, 3, NSA_GROUPS, NSA_HPG).transpose(1, 0, 4, 5, 2, 3)

    def query_block(args):
        c, qc, gc = args
        t_pos = c * C + jnp.arange(C, dtype=jnp.int32)
        s = (jnp.einsum('bghcd,bgnd->bghcn', qc, kc).astype(f32)
             + table[:, :, rel_bucket(t_pos[:, None] - cmp_end[None, :])])
        p_cmp = masked_softmax(s, cmp_end[None, :] <= t_pos[:, None])
        o_cmp = jnp.einsum('bghcn,bgnd->bghcd', p_cmp, vc)
        imp = jnp.einsum('bgcn,nj->bgcj', p_cmp.sum(axis=2), overlap)
        q_blk = t_pos[:, None] // SLC_LEN
        j = jnp.arange(n_slc)[None, :]
        forced = (j == 0) | (j == q_blk) | (j == q_blk - 1)
        imp = jnp.where(forced, BIG, jnp.where(j > q_blk, -BIG, imp))
        _, sel = lax.top_k(imp, top_k)
        k_sel = gather(ks_blk, sel).reshape(B, NSA_GROUPS, C, L, NSA_DH)
        v_sel = gather(vs_blk, sel).reshape(B, NSA_GROUPS, C, L, NSA_DH)
        pos_sel = (sel[..., None] * SLC_LEN + jnp.arange(SLC_LEN, dtype=jnp.int32)).reshape(B, NSA_GROUPS, C, L)
        dist = t_pos[:, None] - pos_sel
        s = (jnp.einsum('bghcd,bgcld->bghcl', qc, k_sel).astype(f32)
             + table[g_idx, h_idx, rel_bucket(dist)[:, :, None]])
        p = masked_softmax(s, (dist >= 0)[:, :, None])
        o_slc = jnp.einsum('bghcl,bgcld->bghcd', p, v_sel)
        kw = lax.dynamic_slice_in_dim(kw_pad, c * C, WIN + C, axis=2)
        vw = lax.dynamic_slice_in_dim(vw_pad, c * C, WIN + C, axis=2)
        valid = band & (jnp.arange(WIN + C, dtype=jnp.int32)[None, :] >= WIN - c * C)
        s = jnp.einsum('bghcd,bgld->bghcl', qc, kw).astype(f32) + bias_w
        p = masked_softmax(s, valid)
        o_win = jnp.einsum('bghcl,bgld->bghcd', p, vw)
        return gc[..., 0:1] * o_cmp + gc[..., 1:2] * o_slc + gc[..., 2:3] * o_win

    o = lax.map(query_block, (jnp.arange(nq, dtype=jnp.int32), qb, gb))
    return o.transpose(1, 0, 4, 2, 3, 5).reshape(B, S, NSA_WIDTH)


def hier_moe(h, w_rg, b_rg, w_re, b_re, w_gate, w_up, w_down):
    B, S, D = h.shape
    T = B * S
    A = T * EXPERT_TOPK
    f32 = jnp.float32
    xt = h.reshape(T, D)
    p_grp = jax.nn.softmax((xt @ w_rg).astype(f32) + b_rg.astype(f32), axis=-1)
    p_top, g_top = lax.top_k(p_grp, 1)
    le = ((xt @ w_re).astype(f32) + b_re.astype(f32)).reshape(T, N_EXPERT_GROUPS, EXPERTS_PER_GROUP)
    le = le[jnp.arange(T), g_top[:, 0]]
    top_v, top_i = lax.top_k(le, EXPERT_TOPK)
    gate = p_top * jax.nn.softmax(top_v, axis=-1)
    e_flat = (g_top * EXPERTS_PER_GROUP + top_i).reshape(A)
    tok_flat = jnp.arange(A, dtype=jnp.int32) // EXPERT_TOPK
    w_flat = gate.reshape(A)
    order = jnp.argsort(e_flat)
    e_s, tok_s, w_s = e_flat[order], tok_flat[order], w_flat[order]
    counts = jnp.bincount(e_flat, length=N_EXPERTS)
    offs = jnp.cumsum(counts) - counts
    padded = (counts + MOE_BLOCK - 1) // MOE_BLOCK * MOE_BLOCK
    pend = jnp.cumsum(padded)
    poffs = pend - padded
    dest = poffs[e_s] + (jnp.arange(A, dtype=jnp.int32) - offs[e_s])
    P = (A + MOE_BLOCK - 1) // MOE_BLOCK * MOE_BLOCK + N_EXPERTS * MOE_BLOCK
    n_blocks = P // MOE_BLOCK
    tok_buf = jnp.zeros((P,), jnp.int32).at[dest].set(tok_s)
    w_buf = jnp.zeros((P,), h.dtype).at[dest].set(w_s.astype(h.dtype))
    blk_start = jnp.arange(n_blocks, dtype=jnp.int32) * MOE_BLOCK
    blk_expert = jnp.minimum(jnp.searchsorted(pend, blk_start, side='right'), N_EXPERTS - 1)
    x_buf = xt[tok_buf].reshape(n_blocks, MOE_BLOCK, D)

    def expert_block(args):
        xb, e = args
        hid = jax.nn.silu(xb @ w_gate[e]) * (xb @ w_up[e])
        return hid @ w_down[e]

    y = lax.map(expert_block, (x_buf, blk_expert)).reshape(P, D)
    out = jnp.zeros((T, D), h.dtype).at[tok_buf].add((y * w_buf[:, None]).astype(h.dtype))
    return out.reshape(B, S, D)


def setup_inputs(seed: int = 0) -> dict:
    key = jax.random.key(seed)
    ks = jax.random.split(key, 24)
    f32 = jnp.float32

    def nrm(k, shape, scale):
        return jax.random.normal(k, shape, f32) * scale

    def gain(k, shape):
        return 1.0 + 0.02 * jax.random.normal(k, shape, f32)

    cmp_in = CMP_LEN * NSA_DH
    return {
        "x": nrm(ks[0], (BATCH, SEQ, D_MODEL), 1.0),
        "norm1_g": gain(ks[1], (DEPTH, D_MODEL)),
        "w_in": nrm(ks[2], (DEPTH, D_MODEL, IN_COLS), D_MODEL ** -0.5),
        "hg_lb_logits": nrm(ks[3], (DEPTH + 1, HG_FDIM), 0.5),
        "hg_norm_g": gain(ks[4], (DEPTH, HG_VDIM)),
        "cmp_pe_k": nrm(ks[5], (DEPTH, CMP_LEN, NSA_DH), 0.1),
        "cmp_w1_k": nrm(ks[6], (DEPTH, cmp_in, CMP_HIDDEN), cmp_in ** -0.5),
        "cmp_w2_k": nrm(ks[7], (DEPTH, CMP_HIDDEN, NSA_DH), CMP_HIDDEN ** -0.5),
        "cmp_pe_v": nrm(ks[8], (DEPTH, CMP_LEN, NSA_DH), 0.1),
        "cmp_w1_v": nrm(ks[9], (DEPTH, cmp_in, CMP_HIDDEN), cmp_in ** -0.5),
        "cmp_w2_v": nrm(ks[10], (DEPTH, CMP_HIDDEN, NSA_DH), CMP_HIDDEN ** -0.5),
        "rel_bias": nrm(ks[11], (REL_BUCKETS, NSA_HEADS), 0.5),
        "w_out": nrm(ks[12], (DEPTH, MIX_WIDTH, D_MODEL), MIX_WIDTH ** -0.5),
        "norm2_g": gain(ks[13], (DEPTH, D_MODEL)),
        "w_router_group": nrm(ks[14], (DEPTH, D_MODEL, N_EXPERT_GROUPS), D_MODEL ** -0.5),
        "b_router_group": nrm(ks[15], (DEPTH, N_EXPERT_GROUPS), 0.01),
        "w_router_expert": nrm(ks[16], (DEPTH, D_MODEL, N_EXPERTS), D_MODEL ** -0.5),
        "b_router_expert": nrm(ks[17], (DEPTH, N_EXPERTS), 0.01),
        "w_expert_gate": nrm(ks[18], (DEPTH, N_EXPERTS, D_MODEL, EXPERT_DFF), D_MODEL ** -0.5),
        "w_expert_up": nrm(ks[19], (DEPTH, N_EXPERTS, D_MODEL, EXPERT_DFF), D_MODEL ** -0.5),
        "w_expert_down": nrm(ks[20], (DEPTH, N_EXPERTS, EXPERT_DFF, D_MODEL), EXPERT_DFF ** -0.5),
        "final_norm_g": gain(ks[21], (D_MODEL,)),
    }


def reference(x, norm1_g, w_in, hg_lb_logits, hg_norm_g, cmp_pe_k, cmp_w1_k, cmp_w2_k,
              cmp_pe_v, cmp_w1_v, cmp_w2_v, rel_bias, w_out, norm2_g, w_router_group,
              b_router_group, w_router_expert, b_router_expert, w_expert_gate, w_expert_up,
              w_expert_down, final_norm_g):
    lower_bounds = jnp.cumsum(jax.nn.softmax(hg_lb_logits.astype(jnp.float32), axis=0), axis=0)
    split_at = np.cumsum(IN_SIZES)[:-1].tolist()
    for l in range(DEPTH):
        h = rmsnorm(x, norm1_g[l])
        (hq, hf, hi, hgate, nsa_q, kcm, vcm, ksl, vsl, kwn, vwn, nsa_gate) = jnp.split(
            h @ w_in[l], split_at, axis=-1)
        y_hg = hgrn2_mixer(hq, hf, hi, hgate, lower_bounds[l], hg_norm_g[l])
        y_nsa = nsa_mixer(nsa_q, kcm, vcm, ksl, vsl, kwn, vwn, nsa_gate,
                          cmp_pe_k[l], cmp_w1_k[l], cmp_w2_k[l],
                          cmp_pe_v[l], cmp_w1_v[l], cmp_w2_v[l], rel_bias)
        mix = jnp.concatenate([y_hg, y_nsa], axis=-1).astype(x.dtype)
        x = x + mix @ w_out[l]
        x = x + hier_moe(rmsnorm(x, norm2_g[l]), w_router_group[l], b_router_group[l],
                         w_router_expert[l], b_router_expert[l],
                         w_expert_gate[l], w_expert_up[l], w_expert_down[l])
    return rmsnorm(x, final_norm_g)
```

```python
import numpy as np
from contextlib import ExitStack
import concourse.bass as bass
import concourse.mybir as mybir
from concourse.bass_utils import run_bass_kernel_spmd

F32 = mybir.dt.float32
BF16 = mybir.dt.bfloat16
I32 = mybir.dt.int32
AF = mybir.ActivationFunctionType
ALU = mybir.AluOpType
AX = mybir.AxisListType

SAME_ENGINE_SYNC = True


class Buf:
    def __init__(self, name, t=None):
        self.name = name
        self.t = t
        self.lw = None
        self.rd = []
        self.excl = False

    def __getitem__(self, k):
        return self.t[k]


class Op:
    __slots__ = ("eng", "fn", "deps", "dma", "sem", "val", "need", "phase")

    def __init__(self, eng, fn, dma):
        self.eng = eng
        self.fn = fn
        self.dma = dma
        self.deps = []
        self.sem = None
        self.val = 0
        self.need = False
        self.phase = 0


class Sched:
    ENG = ("pe", "act", "dve", "pool", "sp")

    def __init__(self, nc, n_dma_sems=10):
        self.nc = nc
        self.es = ExitStack()
        self.ops = {e: [] for e in self.ENG}
        self.sems = {e: self.es.enter_context(nc.semaphore("sem_" + e)) for e in self.ENG}
        self.dsems = {}
        self.dcnt = {}
        self.dlast = {}
        self.drr = {}
        for q in ("sp", "pool", "act"):
            self.dsems[q] = [self.es.enter_context(nc.semaphore("dsem_%s_%d" % (q, i)))
                             for i in range(n_dma_sems)]
            self.dcnt[q] = [0] * n_dma_sems
            self.dlast[q] = [None] * n_dma_sems
            self.drr[q] = 0
        self.phase = 0
        self.ecnt = {e: 0 for e in self.ENG}
        self.ecnt_next = {e: 0 for e in self.ENG}
        self.dcnt_prev = {q: [0] * n_dma_sems for q in self.dsems}
        self.bes = ExitStack()

    def sb(self, name, shape, dtype):
        t = self.bes.enter_context(self.nc.sbuf_tensor("s%d_%s" % (self.phase, name), list(shape), dtype))
        return Buf(name, t)

    def sbp(self, name, shape, dtype):
        t = self.es.enter_context(self.nc.sbuf_tensor("sp_" + name, list(shape), dtype))
        return Buf(name, t)

    def ps(self, name, shape, dtype=F32):
        t = self.bes.enter_context(self.nc.psum_tensor("p%d_%s" % (self.phase, name), list(shape), dtype))
        b = Buf(name, t)
        b.excl = True
        return b

    def sbn(self, name, n, shape, dtype):
        return [self.sb("%s_%d" % (name, i), shape, dtype) for i in range(n)]

    def psn(self, name, n, shape, dtype=F32):
        return [self.ps("%s_%d" % (name, i), shape, dtype) for i in range(n)]

    def tok(self, name):
        return Buf(name, None)

    def _rec(self, eng, fn, R, W, dma=False):
        op = Op(eng, fn, dma)
        op.phase = self.phase
        deps = []
        for r in R:
            if r.lw is not None:
                deps.append(r.lw)
            if r.excl:
                deps.extend(o for o in r.rd if o.eng != eng)
        for w in W:
            if w.lw is not None:
                deps.append(w.lw)
            deps.extend(w.rd)
        for w in W:
            w.lw = op
            w.rd = []
        for r in R:
            if r.lw is not op:
                r.rd.append(op)
        seen = set()
        for d in deps:
            if id(d) not in seen and d is not op:
                seen.add(id(d))
                op.deps.append(d)
        self.ops[eng].append(op)
        return op

    def pe(self, fn, R=(), W=()):
        return self._rec("pe", fn, R, W)

    def act(self, fn, R=(), W=()):
        return self._rec("act", fn, R, W)

    def dve(self, fn, R=(), W=()):
        return self._rec("dve", fn, R, W)

    def pool(self, fn, R=(), W=()):
        return self._rec("pool", fn, R, W)

    def mm(self, out, lhsT, rhs, start=True, stop=True, R=(), W=(), skip=False):
        if skip:
            return self._rec("pe", lambda e: e.matmul(out, lhsT, rhs, start=start, stop=stop,
                                                      skip_group_check=True), R, W)
        return self._rec("pe", lambda e: e.matmul(out, lhsT, rhs, start=start, stop=stop), R, W)

    def tr(self, out, in_, ident, R=(), W=()):
        return self._rec("pe", lambda e: e.transpose(out, in_, ident), R, W)

    def dma(self, q, out, in_, R=(), W=(), out_dram=False, **kw):
        eng = "sp" if q == "sp" else q
        op = self._rec(eng, lambda e: e.dma_start(out=out, in_=in_, **kw), R, W, dma=True)
        i = self.drr[q]
        self.drr[q] = (i + 1) % len(self.dsems[q])
        prev = self.dlast[q][i]
        if prev is not None:
            op.deps.append(prev)
        self.dcnt[q][i] += 1
        self.dlast[q][i] = op
        op.sem = self.dsems[q][i]
        op.val = 16 * self.dcnt[q][i]
        return op

    @staticmethod
    def _needs_wait(op, d):
        if d.dma:
            return True
        if d.eng == op.eng:
            return SAME_ENGINE_SYNC and op.eng != "pe"
        return True

    def begin(self):
        self.bes = ExitStack()

    def flush(self, final=False):
        ph = self.phase
        for e in self.ENG:
            for op in self.ops[e]:
                for d in op.deps:
                    if d.phase == ph and self._needs_wait(op, d):
                        d.need = True
        for e in self.ENG:
            for op in reversed(self.ops[e]):
                if not op.dma:
                    op.need = True
                    break
        for e in self.ENG:
            c = self.ecnt[e]
            for op in self.ops[e]:
                if not op.dma and op.need:
                    c += 1
                    op.sem = self.sems[e]
                    op.val = c
            self.ecnt_next[e] = c
        base = {}
        if ph > 0:
            for e in self.ENG:
                if self.ecnt[e]:
                    base[id(self.sems[e])] = (self.sems[e], self.ecnt[e])
            for q in self.dsems:
                for s, c in zip(self.dsems[q], self.dcnt_prev[q]):
                    if c:
                        base[id(s)] = (s, 16 * c)
        with self.nc.Block() as block:
            @block.sync
            def _(eng):
                self._run("sp", eng, base)
                if final:
                    for q in self.dsems:
                        for s, c in zip(self.dsems[q], self.dcnt[q]):
                            if c:
                                eng.wait_ge(s, 16 * c)

            @block.scalar
            def _(eng):
                self._run("act", eng, base)

            @block.vector
            def _(eng):
                self._run("dve", eng, base)

            @block.gpsimd
            def _(eng):
                self._run("pool", eng, base)

            @block.tensor
            def _(eng):
                self._run("pe", eng, base)
        for e in self.ENG:
            self.ecnt[e] = self.ecnt_next[e]
            self.ops[e] = []
        for q in self.dsems:
            self.dcnt_prev[q] = list(self.dcnt[q])
        self.phase += 1
        self.bes.close()
        if final:
            self.es.close()

    def _run(self, e, eng, base):
        known = {}
        for k, (s, v) in base.items():
            eng.wait_ge(s, v)
            known[k] = v
        ph = self.phase
        for op in self.ops[e]:
            waits = {}
            for d in op.deps:
                if d.phase != ph or not self._needs_wait(op, d):
                    continue
                k = id(d.sem)
                if known.get(k, 0) >= d.val:
                    continue
                if k not in waits or waits[k][1] < d.val:
                    waits[k] = (d.sem, d.val)
            for k, (s, v) in waits.items():
                eng.wait_ge(s, v)
                known[k] = v
            ins = op.fn(eng)
            if op.dma:
                ins.then_inc(op.sem, 16)
            elif op.need:
                ins.then_inc(op.sem, 1)


EPS = 1e-6
TT = 512


def bcast(ap, shape):
    return ap.to_broadcast(list(shape))


class Front:
    def __init__(self, S, D, ps_tp, half_tp=False, nxt=2):
        self.half_tp = half_tp
        self.S = S
        self.D = D
        self.ident = S.sb("ident", [128, 128], BF16)
        S.dma("pool", self.ident[:], D["ident"], W=[self.ident])
        self.g1 = S.sb("g1", [128, 16], F32)
        S.dma("sp", self.g1[:], D["g1"], W=[self.g1])
        self.eps = S.sb("epsT", [128, 1], F32)
        S.dve(lambda e: e.memset(self.eps[:], EPS), W=[self.eps])
        self.xt = S.sbn("xt", nxt, [128, 2048], F32)
        self.xn = S.sb("xn", [128, 2048], BF16)
        self.ss = S.sbn("ss", 2, [128, 1], F32)
        self.rs = S.sbn("rs", 2, [128, 1], F32)
        self.hT = S.sb("hT", [128, 16, TT], BF16)
        self.tp = ps_tp
        self.n = 0

    def tile(self, T):
        S = self.S
        x = self.D["x"]
        for sub in range(4):
            i = self.n % 2
            self.n += 1
            xt, ss, rs, xn, tp, hT = self.xt[i % len(self.xt)], self.ss[i], self.rs[i], self.xn, self.tp, self.hT
            r0 = T * TT + sub * 128
            S.dma("sp", xt[:], x[r0:r0 + 128, :], W=[xt])
            S.act(lambda e, xt=xt, ss=ss, xn=xn: e.activation(out=xn[:], in_=xt[:], func=AF.Square,
                                                               accum_out=ss[:]), R=[xt], W=[xn, ss])
            S.act(lambda e, ss=ss, rs=rs: e.activation(out=rs[:], in_=ss[:], func=AF.Sqrt,
                                                       scale=1.0 / 2048, bias=self.eps[:]),
                  R=[ss, self.eps], W=[rs])
            S.dve(lambda e, rs=rs: e.reciprocal(out=rs[:], in_=rs[:]), R=[rs], W=[rs])
            S.dve(lambda e, xt=xt, rs=rs, xn=xn: e.tensor_scalar(out=xn[:], in0=xt[:], scalar1=rs[:, 0:1],
                                                                  scalar2=None, op0=ALU.mult),
                  R=[xt, rs], W=[xn])
            nh = 2 if self.half_tp else 1
            kper = 16 // nh
            for h_ in range(nh):
                for kk_ in range(kper):
                    kc = h_ * kper + kk_
                    S.tr(tp[:, kk_ * 128:(kk_ + 1) * 128], xn[:, kc * 128:(kc + 1) * 128], self.ident[:],
                         R=[xn, self.ident], W=[tp])
                S.dve(lambda e, sub=sub, h_=h_, kper=kper: e.tensor_tensor(
                    out=hT[:, h_ * kper:(h_ + 1) * kper, sub * 128:(sub + 1) * 128],
                    in0=tp[:, 0:kper * 128].rearrange("p (k t) -> p k t", t=128),
                    in1=bcast(self.g1[:, h_ * kper:(h_ + 1) * kper].unsqueeze(2), [128, kper, 128]), op=ALU.mult),
                    R=[tp, self.g1], W=[hT])


def p1a(S, D, NT=8):
    S.begin()
    tp = S.ps("tp", [128, 2048], BF16)
    pj = S.psn("pj", 2, [128, 512], F32)
    atps = S.ps("atps", [128, 128], F32)
    ops_ = S.ps("ops", [128, 128], F32)
    sups = S.ps("sups", [128, 128], F32)
    kdtr = S.ps("kdtr", [128, 128], BF16)
    fr = Front(S, D, tp)
    w = S.sb("wA", [128, 16, 2048], BF16)
    wtok = [S.tok("w%d" % k) for k in range(16)]
    wv = D["wA"].rearrange("(kc p) c -> p kc c", p=128)
    for kc in range(16):
        S.dma("pool", w[:, kc, :], wv[:, kc, :], W=[wtok[kc]])
    lbl = S.sb("lbl", [128, 8], F32)
    S.dma("sp", lbl[:], D["lbl"], W=[lbl])
    lb = S.sb("lb", [128, 4], F32)
    oml = S.sb("oml", [128, 4], F32)
    noml = S.sb("noml", [128, 4], F32)
    S.dve(lambda e: e.tensor_tensor(out=lb[:], in0=lbl[:, 0:4], in1=lbl[:, 4:8], op=ALU.subtract),
          R=[lbl], W=[lb])
    S.act(lambda e: e.activation(out=lb[:], in_=lb[:], func=AF.Sigmoid), R=[lb], W=[lb])
    S.dve(lambda e: e.tensor_scalar(out=oml[:], in0=lb[:], scalar1=-1.0, scalar2=1.0, op0=ALU.mult,
                                    op1=ALU.add), R=[lb], W=[oml])
    S.dve(lambda e: e.tensor_scalar(out=noml[:], in0=oml[:], scalar1=-1.0, scalar2=None, op0=ALU.mult),
          R=[oml], W=[noml])
    hgn = S.sb("hgn", [128, 128], F32)
    S.dma("sp", hgn[:], D["hgn"], W=[hgn])
    rmask = S.sb("rmask", [128, TT], F32)
    S.dma("sp", rmask[:], D["rmask"], W=[rmask])
    maskbd = S.sb("maskbd", [128, 128], F32)
    S.dma("sp", maskbd[:], D["maskbd"], W=[maskbd])
    Sst = S.sbn("Sst", 4, [128, 128], F32)
    Sbf = S.sbn("Sbf", 4, [128, 128], BF16)
    for hd in range(4):
        S.dve(lambda e, hd=hd: e.memset(Sst[hd][:], 0.0), W=[Sst[hd]])
        S.dve(lambda e, hd=hd: e.memset(Sbf[hd][:], 0.0), W=[Sbf[hd]])
    sq = S.sbn("sq", 4, [128, TT], F32)
    sig = S.sbn("sig", 4, [128, TT], F32)
    logf = S.sb("logf", [128, TT], F32)
    bb = S.sb("bb", [128, TT], F32)
    eb = S.sb("eb", [128, TT], F32)
    enb = S.sb("enb", [128, TT], F32)
    kk = S.sb("kk", [128, TT], F32)
    ebl = S.sbn("ebl", 4, [128, 8], F32)
    qe = S.sbn("qe", 4, [128, TT], BF16)
    ke = S.sbn("ke", 4, [128, TT], BF16)
    kd = S.sbn("kd", 4, [128, TT], BF16)
    vtok = S.sbn("vtok", 4, [128, 512], BF16)
    gsil = S.sbn("gsil", 4, [128, 512], F32)
    AT = S.sbn("AT", 2, [128, 128], BF16)
    kdtok = S.sbn("kdtok", 2, [128, 128], BF16)
    junk = S.sb("junk", [128, 128], F32)
    ssq = S.sbn("ssq", 2, [128, 1], F32)
    mixt = S.sbn("mixt", 2, [128, 512], BF16)
    eps = fr.eps
    hT = fr.hT
    npj = 0
    nat = 0
    for T in range(NT):
        fr.tile(T)
        for ch in range(8):
            p = pj[npj % 2]
            npj += 1
            for kc in range(16):
                S.mm(p[:], w[:, kc, ch * 128:(ch + 1) * 128], hT[:, kc, :], start=(kc == 0), stop=(kc == 15),
                     R=[wtok[kc], hT], W=[p])
            if ch < 4:
                S.act(lambda e, p=p, ch=ch: e.activation(out=sq[ch][:], in_=p[:], func=AF.Silu),
                      R=[p], W=[sq[ch]])
            else:
                S.act(lambda e, p=p, ch=ch: e.activation(out=sig[ch - 4][:], in_=p[:], func=AF.Sigmoid),
                      R=[p], W=[sig[ch - 4]])
        for sub in range(4):
            for grp in range(2):
                p = pj[npj % 2]
                npj += 1
                c0 = 1024 + grp * 512
                for kc in range(16):
                    S.mm(p[:], hT[:, kc, sub * 128:(sub + 1) * 128], w[:, kc, c0:c0 + 512],
                         start=(kc == 0), stop=(kc == 15), R=[wtok[kc], hT], W=[p])
                if grp == 0:
                    S.act(lambda e, p=p, sub=sub: e.activation(out=vtok[sub][:], in_=p[:], func=AF.Copy),
                          R=[p], W=[vtok[sub]])
                else:
                    S.act(lambda e, p=p, sub=sub: e.activation(out=gsil[sub][:], in_=p[:], func=AF.Silu),
                          R=[p], W=[gsil[sub]])
                    S.pool(lambda e, sub=sub: e.tensor_tensor(
                        out=gsil[sub][:].rearrange("p (h v) -> p h v", v=128),
                        in0=gsil[sub][:].rearrange("p (h v) -> p h v", v=128),
                        in1=bcast(hgn[:].unsqueeze(1), [128, 4, 128]), op=ALU.mult),
                        R=[gsil[sub], hgn], W=[gsil[sub]])
        for hd in range(4):
            S.act(lambda e, hd=hd: e.activation(out=logf[:], in_=sig[hd][:], func=AF.Ln,
                                                scale=oml[:, hd:hd + 1], bias=lb[:, hd:hd + 1]),
                  R=[sig[hd], oml, lb], W=[logf])
            S.dve(lambda e: e.tensor_tensor_scan(out=bb[:], data0=rmask[:], data1=logf[:], initial=0.0,
                                                 op0=ALU.mult, op1=ALU.add), R=[rmask, logf], W=[bb])
            S.act(lambda e: e.activation(out=eb[:], in_=bb[:], func=AF.Exp), R=[bb], W=[eb])
            S.act(lambda e: e.activation(out=enb[:], in_=bb[:], func=AF.Exp, scale=-1.0), R=[bb], W=[enb])
            S.dve(lambda e, hd=hd: e.tensor_tensor(out=qe[hd][:], in0=sq[hd][:], in1=eb[:], op=ALU.mult),
                  R=[sq[hd], eb], W=[qe[hd]])
            S.dve(lambda e, hd=hd: e.tensor_copy(out=ebl[hd][:], in_=eb[:, 63::64]), R=[eb], W=[ebl[hd]])
            S.pool(lambda e, hd=hd: e.tensor_scalar(out=kk[:], in0=sig[hd][:], scalar1=noml[:, hd:hd + 1],
                                                    scalar2=oml[:, hd:hd + 1], op0=ALU.mult, op1=ALU.add),
                   R=[sig[hd], noml, oml], W=[kk])
            S.pool(lambda e, hd=hd: e.tensor_tensor(out=ke[hd][:], in0=kk[:], in1=enb[:], op=ALU.mult),
                   R=[kk, enb], W=[ke[hd]])
            S.pool(lambda e, hd=hd: e.tensor_tensor(
                out=kd[hd][:].rearrange("p (c t) -> p c t", t=64),
                in0=ke[hd][:].rearrange("p (c t) -> p c t", t=64),
                in1=bcast(ebl[hd][:].unsqueeze(2), [128, 8, 64]), op=ALU.mult),
                R=[ke[hd], ebl[hd]], W=[kd[hd]])
        for sub in range(4):
            mt = mixt[sub % 2]
            ts = slice(sub * 128, (sub + 1) * 128)
            for hd in range(4):
                a = AT[nat % 2]
                kt = kdtok[nat % 2]
                sq_ = ssq[nat % 2]
                nat += 1
                vs = vtok[sub]
                hc = slice(hd * 128, (hd + 1) * 128)
                S.mm(atps[:], ke[hd][:, ts], qe[hd][:, ts], R=[ke[hd], qe[hd]], W=[atps])
                S.dve(lambda e, a=a: e.tensor_tensor(out=a[:], in0=atps[:], in1=maskbd[:], op=ALU.mult),
                      R=[atps, maskbd], W=[a])
                S.tr(kdtr[:], kd[hd][:, ts], fr.ident[:], R=[kd[hd], fr.ident], W=[kdtr])
                S.act(lambda e, kt=kt: e.activation(out=kt[:], in_=kdtr[:], func=AF.Copy), R=[kdtr], W=[kt])
                for c2 in range(2):
                    ps_ = slice(64 * c2, 64 * c2 + 64)
                    t0 = sub * 128 + 64 * c2
                    ci = sub * 2 + c2
                    S.mm(ops_[ps_, :], qe[hd][:, t0:t0 + 64], Sbf[hd][:], start=True, stop=False,
                         R=[qe[hd], Sbf[hd]], W=[ops_])
                    S.mm(sups[:], kt[ps_, :], vs[ps_, hc], R=[kt, vs], W=[sups])
                    S.dve(lambda e, hd=hd, ci=ci: e.scalar_tensor_tensor(
                        out=Sst[hd][:], in0=Sst[hd][:], scalar=ebl[hd][:, ci:ci + 1], in1=sups[:],
                        op0=ALU.mult, op1=ALU.add), R=[Sst[hd], ebl[hd], sups], W=[Sst[hd]])
                    S.act(lambda e, hd=hd: e.activation(out=Sbf[hd][:], in_=Sst[hd][:], func=AF.Copy),
                          R=[Sst[hd]], W=[Sbf[hd]])
                S.mm(ops_[:], a[:], vs[:, hc], start=False, stop=True, R=[a, vs], W=[ops_])
                S.act(lambda e, sq_=sq_: e.activation(out=junk[:], in_=ops_[:], func=AF.Square,
                                                      accum_out=sq_[:]), R=[ops_], W=[junk, sq_])
                S.act(lambda e, sq_=sq_: e.activation(out=sq_[:], in_=sq_[:], func=AF.Sqrt, scale=1.0 / 128,
                                                      bias=eps[:]), R=[sq_, eps], W=[sq_])
                S.dve(lambda e, sq_=sq_: e.reciprocal(out=sq_[:], in_=sq_[:]), R=[sq_], W=[sq_])
                S.dve(lambda e, sq_=sq_, mt=mt, hc=hc, sub=sub: e.scalar_tensor_tensor(
                    out=mt[:, hc], in0=ops_[:], scalar=sq_[:, 0:1], in1=gsil[sub][:, hc],
                    op0=ALU.mult, op1=ALU.mult), R=[ops_, sq_, gsil[sub]], W=[mt])
            r0 = T * TT + sub * 128
            S.dma("sp", D["mixA"][r0:r0 + 128, :], mt[:], R=[mt])
    S.flush()


def _din(nc, name, shape, dt=F32):
    return nc.dram_tensor(name, list(shape), dt, kind="ExternalInput").ap()


def _dout(nc, name, shape, dt=F32):
    return nc.dram_tensor(name, list(shape), dt, kind="ExternalOutput").ap()


def declare_p1a(nc):
    D = {}
    D["x"] = _din(nc, "x", [4096, 2048])
    D["g1"] = _din(nc, "g1", [128, 16])
    D["wA"] = _din(nc, "wA", [2048, 2048])
    D["lbl"] = _din(nc, "lbl", [128, 8])
    D["hgn"] = _din(nc, "hgn", [128, 128])
    D["ident"] = _din(nc, "ident", [128, 128])
    D["rmask"] = _din(nc, "rmask", [128, TT])
    D["maskbd"] = _din(nc, "maskbd", [128, 128])
    D["mixA"] = _dout(nc, "mixA", [4096, 512], BF16)
    return D


def _consts_a():
    ident = np.eye(128, dtype=np.float32)
    t = np.arange(TT)
    rmask = np.broadcast_to((t % 64 != 0).astype(np.float32)[None, :], (128, TT)).copy()
    s = np.arange(128)
    maskbd = ((s[:, None] // 64 == s[None, :] // 64) & (s[:, None] <= s[None, :])).astype(np.float32)
    return ident, rmask, maskbd


def host_p1a(xb, g1, w_in, lbl, hgn, hh):
    f = np.ascontiguousarray
    cols = np.concatenate([np.arange(512) + off + hh * 512 for off in (0, 1024, 2048, 3072)])
    l = np.concatenate([lbl[0].reshape(8, 128)[4 * hh:4 * hh + 4].T, lbl[1].reshape(8, 128)[4 * hh:4 * hh + 4].T],
                       axis=1)
    ident, rmask, maskbd = _consts_a()
    return {"x": f(xb), "g1": f(g1.reshape(16, 128).T), "wA": f(w_in[:, cols]), "lbl": f(l),
            "hgn": f(np.broadcast_to(hgn[None, :], (128, 128))), "ident": ident, "rmask": rmask,
            "maskbd": maskbd}


NEG = -30000.0
DELTAS = (0, 64, 128, 192)
NCOLB = 1560


def _rel_bucket_np(dist):
    n = np.maximum(dist, 0)
    nf = np.maximum(n, 1).astype(np.float32)
    large = 16 + (np.log(nf / np.float32(16)) / np.float32(np.log(128 / 16)) * np.float32(16)).astype(np.int32)
    large = np.minimum(large, 31)
    return np.where(n < 16, n, large)


def _onehot33(dist):
    b = _rel_bucket_np(dist)
    oh = np.zeros((33,) + dist.shape, np.float32)
    for u in range(32):
        oh[u] = (b == u) & (dist >= 0)
    oh[32] = dist < 0
    return oh


def _consts_b():
    C = {}
    d = np.arange(0, 1000)
    bk = _rel_bucket_np(d)
    far = int(np.max(np.nonzero(bk != 31)[0])) + 1
    assert far <= 129, far
    p = np.arange(128)[None, :]
    i = np.arange(64)[:, None]
    C["ohb"] = np.stack([_onehot33(dl + i - p) for dl in DELTAS], axis=1)
    mp = np.arange(-9, 3)[:, None]
    ii = np.arange(64)[None, :]
    C["ohc"] = _onehot33(ii - 16 * mp - 31)
    wm = []
    for dl in (512, 576):
        dist = dl + np.arange(64)[None, :] - np.arange(128)[:, None]
        m = np.where((dist >= 0) & (dist < 512), 0.0, NEG).astype(np.float32)
        wm.append(np.tile(m[:, None, :], (1, 4, 1)).reshape(128, 256))
    C["wmask"] = np.stack(wm, axis=1)
    ci = np.arange(255)[:, None] * 16
    sj = np.arange(64)[None, :] * 64
    ov = ((ci <= sj + 63) & (ci + 31 >= sj)).astype(np.float32)
    ovp = np.zeros((256, 64), np.float32)
    ovp[:255] = ov
    C["ovl"] = ovp.reshape(2, 128, 64).transpose(1, 0, 2).copy()
    c = np.arange(64)[:, None]
    j = np.arange(64)[None, :]
    fb = np.zeros((64, 64), np.float32)
    fb += 1e9 * ((j == 0).astype(np.float32) + (j == c) + 2.0 * (j == c - 1))
    fb = np.where(j > c, -1e9 * (1.0 + (j - c)), fb).astype(np.float32)
    C["fb"] = np.broadcast_to(fb[None], (64, 64, 64)).copy()
    C["irep"] = np.tile(np.eye(64, dtype=np.float32), (1, 4))
    tm = np.zeros((32, 33), np.float32)
    tm[np.arange(32), np.arange(32)] = 1.0
    tm[31, :32] -= 1.0
    C["tabm"] = tm
    C["ident"] = np.eye(128, dtype=np.float32)
    return C


def declare_p1b(nc):
    D = {}
    D["x"] = _din(nc, "x", [4096, 2048])
    D["g1"] = _din(nc, "g1", [128, 16])
    D["wB"] = _din(nc, "wB", [2048, NCOLB])
    D["tab"] = _din(nc, "tab", [32, 8])
    D["tabm"] = _din(nc, "tabm", [32, 33])
    D["ohb"] = _din(nc, "ohb", [33, 4, 64, 128])
    D["ohc"] = _din(nc, "ohc", [33, 12, 64])
    D["wmask"] = _din(nc, "wmask", [128, 2, 256])
    D["ovl"] = _din(nc, "ovl", [128, 2, 64])
    D["fb"] = _din(nc, "fb", [64, 64, 64])
    D["irep"] = _din(nc, "irep", [64, 256])
    D["ident"] = _din(nc, "ident", [128, 128])
    for kv in "kv":
        D["w1" + kv] = _din(nc, "w1" + kv, [2048, 256])
        D["w2" + kv] = _din(nc, "w2" + kv, [256, 64])
        D["pe" + kv] = _din(nc, "pe" + kv, [128, 16])
    D["mixB"] = _dout(nc, "mixB", [4096, 512], BF16)
    return D


def host_p1b(xb, g1, w_in, rel_bias, cmp, hh):
    f = np.ascontiguousarray
    G = [2 * hh, 2 * hh + 1]
    cols = []
    for j in range(4):
        for g in G:
            h = g * 4 + j
            cols.append(4096 + h * 64 + np.arange(64))
    for off in (5120, 5376):
        for g in G:
            cols.append(off + g * 64 + np.arange(64))
            cols.append(off + g * 64 + np.arange(64))
    for off in (5632, 6144):
        for g in G:
            cols.append(off + g * 64 + np.arange(64))
    for off in (5888, 6400):
        for g in G:
            cols.append(off + g * 64 + np.arange(64))
    for br in range(3):
        for g in G:
            cols.append(6656 + br * 16 + g * 4 + np.arange(4))
    cols = np.concatenate(cols)
    assert cols.size == NCOLB
    C = _consts_b()
    m = {"x": f(xb), "g1": f(g1.reshape(16, 128).T), "wB": f(w_in[:, cols]),
         "tab": f(rel_bias[:, 8 * hh:8 * hh + 8])}
    m.update(C)
    for kv in "kv":
        m["w1" + kv] = f(cmp["w1_" + kv])
        m["w2" + kv] = f(cmp["w2_" + kv])
        pe = cmp["pe_" + kv]
        m["pe" + kv] = f(pe.reshape(16, 2, 64).transpose(1, 2, 0).reshape(128, 16))
    return m


def p1b(S, D, NT=8, stop_at=99):
    S.begin()
    tp = S.ps("tp", [128, 1024], BF16)
    pj = S.psn("pj", 2, [128, 512], F32)
    stp = S.psn("stp", 2, [128, 256], F32)
    ocmp = S.ps("ocmp", [128, 4, 64], F32)
    oslc = S.ps("oslc", [128, 4, 65], F32)
    owin = S.ps("owin", [128, 4, 65], F32)
    fr = Front(S, D, tp, half_tp=True, nxt=1)
    ident = fr.ident
    hT = fr.hT
    w = S.sb("wB", [128, 16, NCOLB], BF16)
    wtok = [S.tok("w%d" % k) for k in range(16)]
    wv = D["wB"].rearrange("(kc p) c -> p kc c", p=128)
    for kc in range(16):
        S.dma("pool", w[:, kc, :], wv[:, kc, :], W=[wtok[kc]])
    def load(name, shape, dt, src, q="pool"):
        b = S.sb(name, shape, dt)
        S.dma(q, b[:], src, W=[b])
        return b
    wmask = load("wmask", [128, 2, 256], BF16, D["wmask"])
    ovl = load("ovl", [128, 2, 64], F32, D["ovl"], "sp")
    irep = load("irep", [64, 256], BF16, D["irep"])
    w1 = {}
    w2 = {}
    pe2 = {}
    for kv in "kv":
        w1[kv] = load("w1" + kv, [128, 16, 256], BF16, D["w1" + kv].rearrange("(lp p) h -> p lp h", p=128))
        w2[kv] = load("w2" + kv, [128, 2, 64], BF16, D["w2" + kv].rearrange("(hf p) d -> p hf d", p=128))
        pe2[kv] = load("pe" + kv, [128, 16], BF16, D["pe" + kv])
    tab = load("tab", [32, 8], F32, D["tab"], "sp")
    tabm = load("tabm", [32, 33], F32, D["tabm"], "sp")
    tabx = S.sb("tabx", [33, 8], BF16)
    S.mm(pj[0][0:33, 0:8], tabm[:], tab[:], R=[tabm, tab], W=[pj[0]])
    S.dve(lambda e: e.memset(tabx[:], NEG), W=[tabx])
    S.act(lambda e: e.activation(out=tabx[0:32, :], in_=pj[0][0:32, 0:8], func=AF.Copy), R=[pj[0]], W=[tabx])
    ohs = S.sb("ohs", [33, 64 * 128], BF16)
    biasT = [[S.sb("biasT%d_%d" % (g, k), [128, 256], BF16) for k in range(4)] for g in range(2)]
    for k in range(4):
        S.dma("pool", ohs[:], D["ohb"][:, k].rearrange("u i p -> u (i p)"), W=[ohs])
        bp = pj[k % 2]
        for i in range(64):
            S.mm(bp[:, 0:512].rearrange("p (h i) -> p h i", i=64)[:, :, i], ohs[:, i * 128:(i + 1) * 128],
                 tabx[:, 0:8], R=[ohs, tabx], W=[bp])
        for g in range(2):
            S.act(lambda e, g=g, k=k, bp=bp: e.activation(out=biasT[g][k][:], in_=bp[:, g * 256:(g + 1) * 256],
                                                          func=AF.Copy), R=[bp], W=[biasT[g][k]])
    ohcs = load("ohcs", [33, 12 * 64], BF16, D["ohc"].rearrange("u m i -> u (m i)"))
    Fp = [S.sb("Fp%d" % k, [128, 507], F32) for k in range(4)]
    fps = pj[0]
    for g in range(2):
        for j in range(4):
            pair = g * 2 + j // 2
            for mi in range(12):
                S.mm(fps[(j % 2) * 64:(j % 2) * 64 + 64, pair * 12 + mi:pair * 12 + mi + 1],
                     ohcs[:, mi * 64:(mi + 1) * 64], tabx[:, g * 4 + j:g * 4 + j + 1],
                     R=[ohcs, tabx], W=[fps])
    for k in range(4):
        S.pool(lambda e, k=k: e.memset(Fp[k][:, 0:243], 0.0), W=[Fp[k]])
        S.pool(lambda e, k=k: e.memset(Fp[k][:, 255:507], NEG), W=[Fp[k]])
        S.act(lambda e, k=k: e.activation(out=Fp[k][:, 243:255], in_=fps[:, k * 12:(k + 1) * 12], func=AF.Copy),
              R=[fps], W=[Fp[k]])
    b1 = {}
    for kv in "kv":
        b1[kv] = S.sb("b1" + kv, [128, 2], F32)
        for hf in range(2):
            for lp in range(16):
                S.mm(pj[1][:, hf:hf + 1], w1[kv][:, lp, hf * 128:(hf + 1) * 128], pe2[kv][:, lp:lp + 1],
                     start=(lp == 0), stop=(lp == 15), R=[w1[kv], pe2[kv]], W=[pj[1]])
        S.act(lambda e, kv=kv: e.activation(out=b1[kv][:], in_=pj[1][:, 0:2], func=AF.Copy), R=[pj[1]],
              W=[b1[kv]])
    if stop_at <= 1:
        S.flush()
        return
    kslT = S.sb("kslT", [128, 4096], BF16)
    kwnT = S.sb("kwnT", [128, 4096], BF16)
    vslx = S.sb("vslx", [128, 32, 2, 66], BF16)
    vwnx = S.sb("vwnx", [128, 32, 2, 66], BF16)
    S.pool(lambda e: e.memset(vslx[:, :, :, 64:65], 1.0), W=[vslx])
    S.pool(lambda e: e.memset(vwnx[:, :, :, 64:65], 1.0), W=[vwnx])
    kcT = S.sb("kcT", [128, 256], BF16)
    vcT = S.sb("vcT", [128, 256], BF16)
    vctok = S.sb("vctok", [128, 2, 128], BF16)
    S.pool(lambda e: e.memset(kcT[:], 0.0), W=[kcT])
    S.pool(lambda e: e.memset(vcT[:], 0.0), W=[vcT])
    x2 = {kv: [S.sb("x2%s%d" % (kv, g), [128, 544], BF16) for g in range(2)] for kv in "kv"}
    for kv in "kv":
        for g in range(2):
            S.pool(lambda e, kv=kv, g=g: e.memset(x2[kv][g][:], 0.0), W=[x2[kv][g]])
    qT = S.sb("qT", [128, 8, 4, 64], BF16)
    gsg = S.sbn("gsg", 4, [128, 24], F32)
    h1a = {kv: [[S.sb("h1a%s%d%d" % (kv, g, hf), [128, 32], BF16) for hf in range(2)] for g in range(2)]
           for kv in "kv"}
    fbt = S.sbn("fbt", 2, [64, 8, 64], F32)
    es = S.sbn("es", 2, [128, 256], F32)
    pb = S.sbn("pb", 2, [128, 256], BF16)
    zc = S.sbn("zc", 2, [128, 1], F32)
    pT = S.sbn("pT", 2, [128, 2, 128], BF16)
    psT = S.sb("psT", [128, 2, 64], F32)
    impm = S.sb("impm", [64, 64], F32)
    impr = S.sb("impr", [64, 64], F32)
    m8 = S.sb("m8", [64, 8], F32)
    m8b = S.sb("m8b", [64, 8], F32)
    negsel = S.sbn("negsel", 2, [64, 64], BF16)
    ptb = S.sbn("ptb", 4, [128, 256], BF16)
    zr = S.sbn("zr", 2, [128, 4], F32)
    cf = S.sbn("cf", 2, [128, 4], F32)
    acc = S.sbn("acc", 2, [128, 4, 64], F32)
    tmp = S.sbn("tmp", 2, [128, 4, 64], F32)
    mixb = S.sbn("mixb", 2, [128, 512], BF16)
    tiny = S.sb("tiny", [128, 1], F32)
    S.dve(lambda e: e.memset(tiny[:], 1e-30), W=[tiny])
    cnt = {"pj": 0, "st": 0, "pt": 0, "blk": 0}

    def nextpj():
        p = pj[cnt["pj"] % 2]
        cnt["pj"] += 1
        return p

    if stop_at <= 1.2:
        S.flush()
        return
    for T in range(NT):
        fr.tile(T)
        if stop_at <= 1.3:
            S.flush()
            return
        S.dma("sp", fbt[T % 2][:], D["fb"][:, 8 * T:8 * T + 8, :], W=[fbt[T % 2]])
        for ch in range(10):
            p = nextpj()
            for kc in range(16):
                S.mm(p[:], w[:, kc, ch * 128:(ch + 1) * 128], hT[:, kc, :], start=(kc == 0), stop=(kc == 15),
                     R=[wtok[kc], hT], W=[p])
            if ch < 4:
                S.act(lambda e, p=p, ch=ch: e.activation(out=qT[:, :, ch, :],
                                                         in_=p[:].rearrange("p (c i) -> p c i", i=64),
                                                         func=AF.Copy, scale=0.125), R=[p], W=[qT])
            elif ch < 8:
                kv = "k" if ch < 6 else "v"
                xb_ = x2[kv][ch % 2]
                S.act(lambda e, p=p, xb_=xb_: e.activation(out=xb_[0:64, 32:544], in_=p[0:64, :], func=AF.Copy),
                      R=[p], W=[xb_])
                S.act(lambda e, p=p, xb_=xb_: e.activation(out=xb_[64:128, 31:543], in_=p[64:128, :],
                                                           func=AF.Copy), R=[p], W=[xb_])
            else:
                dst = kslT if ch == 8 else kwnT
                S.act(lambda e, p=p, dst=dst, T=T: e.activation(out=dst[:, T * TT:(T + 1) * TT], in_=p[:],
                                                                func=AF.Copy), R=[p], W=[dst])
        if stop_at <= 1.4:
            S.flush()
            return
        for sub in range(4):
            p = nextpj()
            kt = T * 4 + sub
            for kc in range(16):
                S.mm(p[:, 0:280], hT[:, kc, sub * 128:(sub + 1) * 128], w[:, kc, 1280:1560],
                     start=(kc == 0), stop=(kc == 15), R=[wtok[kc], hT], W=[p])
            import os as _os
            if not _os.environ.get("SKIPV"):
              S.act(lambda e, p=p, kt=kt: e.activation(out=vslx[:, kt, :, 0:64],
                                                     in_=p[:, 0:128].rearrange("p (g d) -> p g d", d=64),
                                                     func=AF.Copy), R=[p], W=[vslx])
            if not _os.environ.get("SKIPW"):
              S.act(lambda e, p=p, kt=kt: e.activation(out=vwnx[:, kt, :, 0:64],
                                                     in_=p[:, 128:256].rearrange("p (g d) -> p g d", d=64),
                                                     func=AF.Copy), R=[p], W=[vwnx])
            if not _os.environ.get("SKIPG"):
              S.act(lambda e, p=p, sub=sub: e.activation(out=gsg[sub][:], in_=p[:, 256:280], func=AF.Sigmoid),
                  R=[p], W=[gsg[sub]])
        if stop_at <= 1.5:
            S.flush()
            return
        n0 = 0 if T == 0 else 32 * T - 1
        n1 = 32 * T + 30
        NB = n1 - n0 + 1
        col0 = 16 * n0 - 512 * T + 32
        for kv in "kv":
            cp = nextpj()
            for g in range(2):
                for hf in range(2):
                    o = cp[:, (g * 2 + hf) * 32:(g * 2 + hf) * 32 + NB]
                    for lp in range(16):
                        c_ = col0 + 2 * lp
                        S.mm(o, w1[kv][:, lp, hf * 128:(hf + 1) * 128],
                             x2[kv][g][:, c_:c_ + 16 * (NB - 1) + 1:16], start=(lp == 0), stop=(lp == 15),
                             R=[w1[kv], x2[kv][g]], W=[cp])
                    S.act(lambda e, o=o, kv=kv, g=g, hf=hf, NB=NB: e.activation(
                        out=h1a[kv][g][hf][:, 0:NB], in_=o, func=AF.Silu, bias=b1[kv][:, hf:hf + 1]),
                        R=[cp, b1[kv]], W=[h1a[kv][g][hf]])
            for g in range(2):
                S.pool(lambda e, kv=kv, g=g: e.tensor_copy(out=x2[kv][g][:, 0:32], in_=x2[kv][g][:, 512:544]),
                       R=[x2[kv][g]], W=[x2[kv][g]])
            c2 = nextpj()
            dstT = kcT if kv == "k" else vcT
            for g in range(2):
                for hf in range(2):
                    S.mm(c2[g * 64:(g + 1) * 64, 0:NB], w2[kv][:, hf, :], h1a[kv][g][hf][:, 0:NB],
                         start=(hf == 0), stop=(hf == 1), R=[w2[kv], h1a[kv][g][hf]], W=[c2])
            S.act(lambda e, c2=c2, dstT=dstT, n0=n0, NB=NB: e.activation(out=dstT[:, n0:n0 + NB],
                                                                         in_=c2[:, 0:NB], func=AF.Copy),
                  R=[c2], W=[dstT])
        for nt in sorted({n0 // 128, n1 // 128}):
            S.tr(tp[:, 0:128], vcT[:, nt * 128:(nt + 1) * 128], ident[:], R=[vcT, ident], W=[tp])
            S.act(lambda e, nt=nt: e.activation(out=vctok[:, nt, :], in_=tp[:, 0:128], func=AF.Copy),
                  R=[tp], W=[vctok])
        if stop_at <= 2:
            S.flush()
            return
        for cc in range(8):
            c = 8 * T + cc
            sub = cc // 2
            hr = slice((cc % 2) * 64, (cc % 2) * 64 + 64)
            a = c // 2
            N = min(4 * c + 3, 255)
            kns = [min(N, 128)] + ([N - 128] if N > 128 else [])
            mt = mixb[sub % 2]
            for g in range(2):
                gr = slice(g * 64, (g + 1) * 64)
                bi = cnt["blk"] % 2
                cnt["blk"] += 1
                qc = qT[gr, cc, :, :].rearrange("p j i -> p (j i)")
                for pr in range(2):
                    sp_ = nextpj()
                    S.mm(sp_[:, 0:N], qT[gr, cc, 2 * pr:2 * pr + 2, :].rearrange("p j i -> p (j i)"), kcT[gr, 0:N],
                         R=[qT, kcT], W=[sp_])
                    e_, p_, z_ = es[pr], pb[pr], zc[pr]
                    F = Fp[g * 2 + pr]
                    o0 = 252 - 4 * c
                    S.dve(lambda e, sp_=sp_, e_=e_, F=F, o0=o0, N=N: e.tensor_tensor(
                        out=e_[:, 0:N], in0=sp_[:, 0:N], in1=F[:, o0:o0 + N], op=ALU.add),
                        R=[sp_, F], W=[e_])
                    S.act(lambda e, e_=e_, z_=z_, N=N: e.activation(out=e_[:, 0:N], in_=e_[:, 0:N], func=AF.Exp,
                                                                    accum_out=z_[:]), R=[e_], W=[e_, z_])
                    S.dve(lambda e, z_=z_: e.tensor_tensor(out=z_[:], in0=z_[:], in1=tiny[:], op=ALU.max),
                          R=[z_, tiny], W=[z_])
                    S.dve(lambda e, z_=z_: e.reciprocal(out=z_[:], in_=z_[:]), R=[z_], W=[z_])
                    S.dve(lambda e, e_=e_, p_=p_, z_=z_, N=N: e.tensor_scalar(
                        out=p_[:, 0:N], in0=e_[:, 0:N], scalar1=z_[:, 0:1], scalar2=None, op0=ALU.mult),
                        R=[e_, z_], W=[p_])
                    for nt, kn in enumerate(kns):
                        S.tr(tp[0:kn, nt * 128:(nt + 1) * 128], p_[:, nt * 128:nt * 128 + kn], ident[:],
                             R=[p_, ident], W=[tp])
                    for nt, kn in enumerate(kns):
                        S.act(lambda e, pr=pr, nt=nt, kn=kn: e.activation(
                            out=pT[pr][0:kn, nt, :], in_=tp[0:kn, nt * 128:(nt + 1) * 128], func=AF.Copy),
                            R=[tp], W=[pT[pr]])
                for j in range(4):
                    for nt, kn in enumerate(kns):
                        S.mm(ocmp[hr, j, :], pT[j // 2][0:kn, nt, (j % 2) * 64:(j % 2) * 64 + 64],
                             vctok[0:kn, nt, gr], start=(nt == 0), stop=(nt == len(kns) - 1),
                             R=[pT[j // 2], vctok], W=[ocmp])
                if stop_at <= 3:
                    S.flush()
                    return
                nk = len(kns)
                S.pool(lambda e, nk=nk: e.tensor_tensor(out=psT[:, 0:nk, :], in0=pT[0][:, 0:nk, 0:64],
                                                        in1=pT[0][:, 0:nk, 64:128], op=ALU.add),
                       R=[pT[0]], W=[psT])
                S.pool(lambda e, nk=nk: e.tensor_tensor(out=psT[:, 0:nk, :], in0=psT[:, 0:nk, :],
                                                        in1=pT[1][:, 0:nk, 0:64], op=ALU.add),
                       R=[pT[1], psT], W=[psT])
                S.pool(lambda e, nk=nk: e.tensor_tensor(out=psT[:, 0:nk, :], in0=psT[:, 0:nk, :],
                                                        in1=pT[1][:, 0:nk, 64:128], op=ALU.add),
                       R=[pT[1], psT], W=[psT])
                ip = nextpj()
                for nt, kn in enumerate(kns):
                    S.mm(ip[0:64, 0:64], psT[0:kn, nt, :], ovl[0:kn, nt, :], start=(nt == 0),
                         stop=(nt == len(kns) - 1), R=[psT, ovl], W=[ip])
                fbc = fbt[T % 2]
                S.dve(lambda e, ip=ip, fbc=fbc, cc=cc: e.tensor_tensor(out=impm[:], in0=ip[0:64, 0:64],
                                                                       in1=fbc[:, cc, :], op=ALU.add),
                      R=[ip, fbc], W=[impm])
                S.dve(lambda e: e.max(out=m8[:], in_=impm[:]), R=[impm], W=[m8])
                S.dve(lambda e: e.match_replace(out=impr[:], in_to_replace=m8[:], in_values=impm[:],
                                                imm_value=-3.0e38), R=[impm, m8], W=[impr])
                S.dve(lambda e: e.max(out=m8b[:], in_=impr[:]), R=[impr], W=[m8b])
                ns = negsel[bi]
                S.dve(lambda e, ns=ns: e.tensor_scalar(out=ns[:], in0=impm[:], scalar1=m8b[:, 7:8], scalar2=NEG,
                                                       op0=ALU.is_lt, op1=ALU.mult), R=[impm, m8b], W=[ns])
                if stop_at <= 4:
                    S.flush()
                    return
                for br, (kT_, vx, oacc, m_lo) in enumerate(((kslT, vslx, oslc, 0), (kwnT, vwnx, owin, max(0, a - 4)))):
                    for m in range(m_lo, a + 1):
                        st = stp[cnt["st"] % 2]
                        cnt["st"] += 1
                        pt = ptb[cnt["pt"] % 4]
                        cnt["pt"] += 1
                        dl = 64 * c - 128 * m
                        extra = []
                        if br == 0:
                            for blk in range(2):
                                extra.append((ns[:, 2 * m + blk:2 * m + blk + 1].to_broadcast([64, 64]),
                                              irep[:], [ns, irep], slice(blk * 64, blk * 64 + 64)))
                        if dl in DELTAS:
                            bt = biasT[g][DELTAS.index(dl)]
                            extra.append((ident[:], bt[:], [ident, bt], slice(0, 128)))
                        elif br == 1 and dl in (512, 576):
                            extra.append((ident[:], wmask[:, (dl - 512) // 64, :], [ident, wmask], slice(0, 128)))
                        S.mm(st[:], kT_[gr, m * 128:(m + 1) * 128], qc, start=True, stop=(not extra),
                             R=[kT_, qT], W=[st])
                        for xi, (l_, r_, rr, osl) in enumerate(extra):
                            S.mm(st[osl, :], l_, r_, start=False, stop=(xi == len(extra) - 1), R=rr, W=[st])
                        S.act(lambda e, st=st, pt=pt: e.activation(out=pt[:], in_=st[:], func=AF.Exp),
                              R=[st], W=[pt])
                        for j in range(4):
                            S.mm(oacc[hr, j, :], pt[:, j * 64:(j + 1) * 64], vx[:, m, g, 0:65],
                                 start=(m == m_lo and j == 0), stop=(m == a), R=[pt, vx], W=[oacc], skip=True)
                if stop_at <= 5:
                    S.flush()
                    return
                z2, c2_, ac, tm_ = zr[bi], cf[bi], acc[bi], tmp[bi]
                gs_ = gsg[sub]
                S.dve(lambda e, ac=ac, gs_=gs_, g=g, hr=hr: e.tensor_tensor(
                    out=ac[hr], in0=ocmp[hr], in1=gs_[hr, g * 4:g * 4 + 4].unsqueeze(2).to_broadcast([64, 4, 64]),
                    op=ALU.mult), R=[ocmp, gs_], W=[ac])
                for br, oacc in ((1, oslc), (2, owin)):
                    S.dve(lambda e, z2=z2, oacc=oacc, hr=hr: e.reciprocal(out=z2[hr], in_=oacc[hr, :, 64]),
                          R=[oacc], W=[z2])
                    S.dve(lambda e, z2=z2, c2_=c2_, gs_=gs_, br=br, g=g, hr=hr: e.tensor_tensor(
                        out=c2_[hr], in0=z2[hr], in1=gs_[hr, br * 8 + g * 4:br * 8 + g * 4 + 4], op=ALU.mult),
                        R=[z2, gs_], W=[c2_])
                    S.dve(lambda e, tm_=tm_, oacc=oacc, c2_=c2_, hr=hr: e.tensor_tensor(
                        out=tm_[hr], in0=oacc[hr, :, 0:64], in1=c2_[hr].unsqueeze(2).to_broadcast([64, 4, 64]),
                        op=ALU.mult), R=[oacc, c2_], W=[tm_])
                    if br == 1:
                        S.pool(lambda e, ac=ac, tm_=tm_, hr=hr: e.tensor_tensor(out=ac[hr], in0=ac[hr], in1=tm_[hr],
                                                                                op=ALU.add), R=[ac, tm_], W=[ac])
                    else:
                        S.pool(lambda e, ac=ac, tm_=tm_, hr=hr, mt=mt, g=g: e.tensor_tensor(
                            out=mt[hr, g * 256:(g + 1) * 256].rearrange("p (j d) -> p j d", d=64), in0=ac[hr],
                            in1=tm_[hr], op=ALU.add), R=[ac, tm_], W=[mt])
            if cc % 2 == 1:
                r0 = T * TT + sub * 128
                S.dma("sp", D["mixB"][r0:r0 + 128, :], mt[:], R=[mt])
    S.flush()


NTOK = 2048
NT2 = NTOK // 128
CAP = 128


def declare_p23(nc, with_mix=True):
    D = {}
    D["xo"] = _din(nc, "xo", [NTOK, 2048])
    if with_mix:
        D["mix"] = _din(nc, "mix", [NTOK, 2048], BF16)
    D["wout"] = _din(nc, "wout", [2048, 2048])
    D["g2b"] = _din(nc, "g2b", [128, 2048])
    D["gfb"] = _din(nc, "gfb", [128, 2048])
    D["wr"] = _din(nc, "wr", [2048, 72])
    D["brb"] = _din(nc, "brb", [128, 72])
    D["wg"] = _din(nc, "wg", [64, 2048, 1024])
    D["wu"] = _din(nc, "wu", [64, 2048, 1024])
    D["wd"] = _din(nc, "wd", [64, 1024, 2048])
    D["identf"] = _din(nc, "identf", [128, 128])
    D["tris"] = _din(nc, "tris", [128, 128])
    D["ones"] = _din(nc, "ones", [128, 128])
    D["iota"] = _din(nc, "iota", [128, 128])
    D["x1d"] = nc.dram_tensor("x1d", [NTOK, 2048], F32).ap()
    D["parts"] = nc.dram_tensor("parts", [8, NTOK, 2048], BF16).ap()
    D["out"] = _dout(nc, "out", [NTOK, 2048])
    return D


def host_p23(x_rows, w_out, g2, gf, w_rg, b_rg, w_re, b_re, wg, wu, wd):
    f = np.ascontiguousarray
    s = np.arange(128)
    m = {"xo": f(x_rows), "wout": f(w_out), "g2b": f(np.broadcast_to(g2[None, :], (128, 2048))),
         "gfb": f(np.broadcast_to(gf[None, :], (128, 2048))),
         "wr": f(np.concatenate([w_rg, w_re], axis=1)),
         "brb": f(np.broadcast_to(np.concatenate([b_rg, b_re])[None, :], (128, 72))),
         "wg": wg, "wu": wu, "wd": wd,
         "identf": np.eye(128, dtype=np.float32),
         "tris": (s[:, None] < s[None, :]).astype(np.float32),
         "ones": np.ones((128, 128), np.float32),
         "iota": f(np.broadcast_to(s[None, :].astype(np.float32), (128, 128)))}
    return m


def p23(S, D, mix_src=None):
    h2all = S.sbp("h2all", [128, NT2, 2048], BF16)
    Dall = S.sbp("Dall", [128, NT2, 64], F32)
    Wtall = S.sbp("Wtall", [128, NT2, 64], F32)
    Mall = S.sbp("Mall", [128, NT2, 64], BF16)
    S.begin()
    tp = S.ps("tp", [128, 1024], BF16)
    tpf = S.ps("tpf", [128, 512], F32)
    pj = S.psn("pj", 2, [128, 512], F32)
    rps = S.ps("rps", [128, 72], F32)
    pps = S.ps("pps", [128, 64], F32)

    def load(name, shape, dt, src, q="pool"):
        b = S.sb(name, shape, dt)
        S.dma(q, b[:], src, W=[b])
        return b
    identb = load("identb", [128, 128], BF16, D["identf"])
    identf = load("identf", [128, 128], F32, D["identf"], "sp")
    tris = load("tris", [128, 128], BF16, D["tris"])
    ones = load("ones", [128, 128], BF16, D["ones"])
    g2b = load("g2b", [128, 2048], F32, D["g2b"], "sp")
    wr = load("wr", [128, 16, 72], F32, D["wr"].rearrange("(kc p) c -> p kc c", p=128), "sp")
    brb = load("brb", [128, 72], F32, D["brb"], "sp")
    wout = S.sb("wout", [128, 16, 2048], BF16)
    wtok = [S.tok("wo%d" % k) for k in range(16)]
    wv = D["wout"].rearrange("(kc p) c -> p kc c", p=128)
    for kc in range(16):
        S.dma("pool", wout[:, kc, :], wv[:, kc, :], W=[wtok[kc]])
    eps = S.sb("eps", [128, 1], F32)
    S.dve(lambda e: e.memset(eps[:], EPS), W=[eps])
    mixt = S.sbn("mixt", 2, [128, 2048], BF16)
    mixT = S.sb("mixT", [128, 16, 128], BF16)
    xt = S.sbn("xt", 1, [128, 2048], F32)
    x1 = S.sbn("x1", 1, [128, 2048], F32)
    h2f = S.sb("h2f", [128, 2048], F32)
    h2T = S.sb("h2T", [128, 16, 128], F32)
    ss = S.sbn("ss", 2, [128, 1], F32)
    sm = {n: S.sb("r_" + n, [128, w_], F32) for n, w_ in
          (("lgs", 8), ("les", 64), ("gmax", 1), ("ngmax", 1), ("eg", 8), ("zg", 1), ("ptop", 1), ("ohg", 8),
           ("mk8", 8), ("lem", 64), ("m8", 8), ("dv", 1), ("w1", 1), ("g1w", 1), ("g2w", 1), ("oh1", 64),
           ("oh2", 64), ("t1", 64), ("mf", 64), ("mm1", 64), ("tmp", 64))}
    npj = 0
    for T in range(NT2):
        r0 = T * 128
        mt, xt_, x1_, ss_ = mixt[T % 2], xt[0], x1[0], ss[T % 2]
        if mix_src is None:
            S.dma("sp", mt[:], D["mix"][r0:r0 + 128, :], W=[mt])
        else:
            mix_src(S, mt, T)
        S.dma("sp", xt_[:], D["xo"][r0:r0 + 128, :], W=[xt_])
        for h_ in range(2):
            for k_ in range(8):
                kc = h_ * 8 + k_
                S.tr(tp[:, k_ * 128:(k_ + 1) * 128], mt[:, kc * 128:(kc + 1) * 128], identb[:], R=[mt, identb],
                     W=[tp])
            S.act(lambda e, h_=h_: e.activation(out=mixT[:, h_ * 8:(h_ + 1) * 8, :],
                                                in_=tp[:].rearrange("p (k t) -> p k t", t=128), func=AF.Copy),
                  R=[tp], W=[mixT])
        for cc in range(4):
            p = pj[npj % 2]
            npj += 1
            for kc in range(16):
                S.mm(p[:], mixT[:, kc, :], wout[:, kc, cc * 512:(cc + 1) * 512], start=(kc == 0), stop=(kc == 15),
                     R=[mixT, wtok[kc]], W=[p])
            S.dve(lambda e, p=p, cc=cc, x1_=x1_, xt_=xt_: e.tensor_tensor(
                out=x1_[:, cc * 512:(cc + 1) * 512], in0=p[:], in1=xt_[:, cc * 512:(cc + 1) * 512], op=ALU.add),
                R=[p, xt_], W=[x1_])
        S.dma("sp", D["x1d"][r0:r0 + 128, :], x1_[:], R=[x1_])
        S.act(lambda e, x1_=x1_, ss_=ss_: e.activation(out=h2f[:], in_=x1_[:], func=AF.Square, accum_out=ss_[:]),
              R=[x1_], W=[h2f, ss_])
        S.act(lambda e, ss_=ss_: e.activation(out=ss_[:], in_=ss_[:], func=AF.Sqrt, scale=1.0 / 2048, bias=eps[:]),
              R=[ss_, eps], W=[ss_])
        S.dve(lambda e, ss_=ss_: e.reciprocal(out=ss_[:], in_=ss_[:]), R=[ss_], W=[ss_])
        S.dve(lambda e, x1_=x1_, ss_=ss_: e.scalar_tensor_tensor(out=h2f[:], in0=x1_[:], scalar=ss_[:, 0:1],
                                                                 in1=g2b[:], op0=ALU.mult, op1=ALU.mult),
              R=[x1_, ss_, g2b], W=[h2f])
        S.pool(lambda e, T=T: e.tensor_copy(out=h2all[:, T, :], in_=h2f[:]), R=[h2f], W=[h2all])
        for q4 in range(4):
            for k_ in range(4):
                kc = q4 * 4 + k_
                S.tr(tpf[:, k_ * 128:(k_ + 1) * 128], h2f[:, kc * 128:(kc + 1) * 128], identf[:],
                     R=[h2f, identf], W=[tpf])
            S.act(lambda e, q4=q4: e.activation(out=h2T[:, q4 * 4:(q4 + 1) * 4, :],
                                                in_=tpf[:].rearrange("p (k t) -> p k t", t=128), func=AF.Copy),
                  R=[tpf], W=[h2T])
        for kc in range(16):
            S.mm(rps[:], h2T[:, kc, :], wr[:, kc, :], start=(kc == 0), stop=(kc == 15), R=[h2T, wr], W=[rps])
        v = sm
        S.dve(lambda e: e.tensor_tensor(out=v["lgs"][:], in0=rps[:, 0:8], in1=brb[:, 0:8], op=ALU.add),
              R=[rps, brb], W=[v["lgs"]])
        S.dve(lambda e: e.tensor_tensor(out=v["les"][:], in0=rps[:, 8:72], in1=brb[:, 8:72], op=ALU.add),
              R=[rps, brb], W=[v["les"]])
        S.dve(lambda e: e.tensor_reduce(out=v["gmax"][:], in_=v["lgs"][:], axis=AX.X, op=ALU.max),
              R=[v["lgs"]], W=[v["gmax"]])
        S.dve(lambda e: e.tensor_scalar(out=v["ngmax"][:], in0=v["gmax"][:], scalar1=-1.0, scalar2=None,
                                        op0=ALU.mult), R=[v["gmax"]], W=[v["ngmax"]])
        S.act(lambda e: e.activation(out=v["eg"][:], in_=v["lgs"][:], func=AF.Exp, bias=v["ngmax"][:],
                                     accum_out=v["zg"][:]), R=[v["lgs"], v["ngmax"]], W=[v["eg"], v["zg"]])
        S.dve(lambda e: e.reciprocal(out=v["ptop"][:], in_=v["zg"][:]), R=[v["zg"]], W=[v["ptop"]])
        S.dve(lambda e: e.tensor_scalar(out=v["ohg"][:], in0=v["lgs"][:], scalar1=v["gmax"][:, 0:1], scalar2=None,
                                        op0=ALU.is_equal), R=[v["lgs"], v["gmax"]], W=[v["ohg"]])
        S.dve(lambda e: e.tensor_scalar(out=v["mk8"][:], in0=v["ohg"][:], scalar1=-1.0, scalar2=1e9, op0=ALU.add,
                                        op1=ALU.mult), R=[v["ohg"]], W=[v["mk8"]])
        S.dve(lambda e: e.tensor_tensor(out=v["lem"][:].rearrange("p (g x) -> p g x", x=8),
                                        in0=v["les"][:].rearrange("p (g x) -> p g x", x=8),
                                        in1=v["mk8"][:].unsqueeze(2).to_broadcast([128, 8, 8]), op=ALU.add),
              R=[v["les"], v["mk8"]], W=[v["lem"]])
        S.dve(lambda e: e.max(out=v["m8"][:], in_=v["lem"][:]), R=[v["lem"]], W=[v["m8"]])
        S.dve(lambda e: e.tensor_tensor(out=v["dv"][:], in0=v["m8"][:, 0:1], in1=v["m8"][:, 1:2], op=ALU.subtract),
              R=[v["m8"]], W=[v["dv"]])
        S.act(lambda e: e.activation(out=v["w1"][:], in_=v["dv"][:], func=AF.Sigmoid), R=[v["dv"]], W=[v["w1"]])
        S.dve(lambda e: e.tensor_tensor(out=v["g1w"][:], in0=v["ptop"][:], in1=v["w1"][:], op=ALU.mult),
              R=[v["ptop"], v["w1"]], W=[v["g1w"]])
        S.dve(lambda e: e.tensor_tensor(out=v["g2w"][:], in0=v["ptop"][:], in1=v["g1w"][:], op=ALU.subtract),
              R=[v["ptop"], v["g1w"]], W=[v["g2w"]])
        S.dve(lambda e: e.tensor_scalar(out=v["oh1"][:], in0=v["lem"][:], scalar1=v["m8"][:, 0:1], scalar2=None,
                                        op0=ALU.is_equal), R=[v["lem"], v["m8"]], W=[v["oh1"]])
        S.dve(lambda e: e.tensor_scalar(out=v["oh2"][:], in0=v["lem"][:], scalar1=v["m8"][:, 1:2], scalar2=None,
                                        op0=ALU.is_equal), R=[v["lem"], v["m8"]], W=[v["oh2"]])
        S.dve(lambda e: e.tensor_scalar(out=v["t1"][:], in0=v["oh1"][:], scalar1=v["g1w"][:, 0:1], scalar2=None,
                                        op0=ALU.mult), R=[v["oh1"], v["g1w"]], W=[v["t1"]])
        S.dve(lambda e, T=T: e.scalar_tensor_tensor(out=Wtall[:, T, :], in0=v["oh2"][:], scalar=v["g2w"][:, 0:1],
                                                    in1=v["t1"][:], op0=ALU.mult, op1=ALU.add),
              R=[v["oh2"], v["g2w"], v["t1"]], W=[Wtall])
        S.dve(lambda e: e.tensor_tensor(out=v["mf"][:], in0=v["oh1"][:], in1=v["oh2"][:], op=ALU.add),
              R=[v["oh1"], v["oh2"]], W=[v["mf"]])
        S.dve(lambda e, T=T: e.tensor_copy(out=Mall[:, T, :], in_=v["mf"][:]), R=[v["mf"]], W=[Mall])
        S.dve(lambda e: e.tensor_scalar(out=v["mm1"][:], in0=v["mf"][:], scalar1=-1.0, scalar2=None, op0=ALU.add),
              R=[v["mf"]], W=[v["mm1"]])
        S.mm(pps[:], tris[:], Mall[:, T, :], start=True, stop=(T == 0), R=[tris, Mall], W=[pps])
        for Tp in range(T):
            S.mm(pps[:], ones[:], Mall[:, Tp, :], start=False, stop=(Tp == T - 1), R=[ones, Mall], W=[pps])
        S.dve(lambda e: e.tensor_tensor(out=v["tmp"][:], in0=pps[:], in1=v["mf"][:], op=ALU.mult),
              R=[pps, v["mf"]], W=[v["tmp"]])
        S.dve(lambda e, T=T: e.tensor_tensor(out=Dall[:, T, :], in0=v["tmp"][:], in1=v["mm1"][:], op=ALU.add),
              R=[v["tmp"], v["mm1"]], W=[Dall])
    S.flush()
    S.begin()
    tp = S.ps("tp", [128, 1024], BF16)
    gps = S.psn("gps", 2, [128, 4, 128], F32)
    gups = S.psn("gups", 2, [128, 2, 128], F32)
    dps = S.psn("dps", 2, [128, 512], F32)
    cps = S.ps("cps", [128, 512], F32)
    identb = load("identb", [128, 128], BF16, D["identf"])
    iota = load("iota", [128, 128], F32, D["iota"], "sp")
    OHs = S.sbn("OHs", 1, [128, NT2, 128], BF16)
    OHw = S.sbn("OHw", 2, [128, 128], BF16)
    ohwT = S.sbn("ohwT", 8, [128, NT2, 128], BF16)
    xg = S.sbn("xg", 1, [128, 16, 128], BF16)
    wgb = S.sbn("wgb", 2, [128, 16, 256], BF16)
    wub = S.sbn("wub", 2, [128, 16, 256], BF16)
    wdb = S.sbn("wdb", 2, [128, 8, 512], BF16)
    hidT = S.sbn("hidT", 2, [128, 8, 128], BF16)
    yb = S.sbn("yb", 8, [128, 2048], BF16)
    sg = S.sbn("sg", 2, [128, 128], F32)
    part = S.sbn("part", 1, [128, 2048], BF16)
    cw = {"g": 0, "d": 0, "gu": 0, "dp": 0, "oh": 0, "pt": 0}
    for G in range(8):
        for el in range(8):
            ex = G * 8 + el
            oh = OHs[0]
            for T in range(NT2):
                eng = S.dve if T % 2 == 0 else S.pool
                eng(lambda e, T=T, ex=ex, oh=oh: e.tensor_scalar(
                    out=oh[:, T, :], in0=iota[:], scalar1=Dall[:, T, ex:ex + 1], scalar2=None, op0=ALU.is_equal),
                    R=[iota, Dall], W=[oh])
                ow = OHw[cw["oh"] % 2]
                cw["oh"] += 1
                eng2 = S.pool if T % 2 == 0 else S.dve
                eng2(lambda e, T=T, ex=ex, ow=ow: e.tensor_scalar(
                    out=ow[:], in0=iota[:], scalar1=Dall[:, T, ex:ex + 1], scalar2=Wtall[:, T, ex:ex + 1],
                    op0=ALU.is_equal, op1=ALU.mult), R=[iota, Dall, Wtall], W=[ow])
                S.tr(tp[:, 0:128], ow[:], identb[:], R=[ow, identb], W=[tp])
                S.act(lambda e, T=T, el=el: e.activation(out=ohwT[el][:, T, :], in_=tp[:, 0:128], func=AF.Copy),
                      R=[tp], W=[ohwT[el]])
            xg_ = xg[0]
            for half in range(2):
                for T in range(NT2):
                    for k_ in range(8):
                        kc = half * 8 + k_
                        S.mm(gps[k_ // 4][:, k_ % 4, :], h2all[:, T, kc * 128:(kc + 1) * 128], oh[:, T, :],
                             start=(T == 0 and k_ % 4 == 0), stop=(T == NT2 - 1), R=[h2all, oh], W=[gps[k_ // 4]],
                             skip=True)
                for b_ in range(2):
                    S.act(lambda e, b_=b_, half=half, xg_=xg_: e.activation(
                        out=xg_[:, half * 8 + b_ * 4:half * 8 + b_ * 4 + 4, :], in_=gps[b_][:], func=AF.Copy),
                        R=[gps[b_]], W=[xg_])
            hid = hidT[ex % 2]
            for fq in range(4):
                wg_, wu_ = wgb[cw["g"] % 2], wub[cw["g"] % 2]
                cw["g"] += 1
                S.dma("pool", wg_[:], D["wg"][ex, :, fq * 256:(fq + 1) * 256].rearrange("(kc p) f -> p kc f", p=128),
                      W=[wg_])
                S.dma("pool", wu_[:], D["wu"][ex, :, fq * 256:(fq + 1) * 256].rearrange("(kc p) f -> p kc f", p=128),
                      W=[wu_])
                for fh in range(2):
                    fc = fq * 2 + fh
                    gu = gups[cw["gu"] % 2]
                    sg_ = sg[cw["gu"] % 2]
                    cw["gu"] += 1
                    for i_, wb in enumerate((wg_, wu_)):
                        for kc in range(16):
                            S.mm(gu[:, i_, :], wb[:, kc, fh * 128:(fh + 1) * 128], xg_[:, kc, :], start=(kc == 0),
                                 stop=(kc == 15), R=[wb, xg_], W=[gu], skip=True)
                    S.act(lambda e, gu=gu, sg_=sg_: e.activation(out=sg_[:], in_=gu[:, 0, :], func=AF.Silu),
                          R=[gu], W=[sg_])
                    S.dve(lambda e, gu=gu, sg_=sg_, hid=hid, fc=fc: e.tensor_tensor(
                        out=hid[:, fc, :], in0=sg_[:], in1=gu[:, 1, :], op=ALU.mult), R=[gu, sg_], W=[hid])
            for cc in range(4):
                wd_ = wdb[cw["d"] % 2]
                cw["d"] += 1
                S.dma("pool", wd_[:], D["wd"][ex, :, cc * 512:(cc + 1) * 512].rearrange("(fc p) c -> p fc c", p=128),
                      W=[wd_])
                dp = dps[cw["dp"] % 2]
                cw["dp"] += 1
                for fc in range(8):
                    S.mm(dp[:], hid[:, fc, :], wd_[:, fc, :], start=(fc == 0), stop=(fc == 7), R=[hid, wd_], W=[dp])
                S.act(lambda e, dp=dp, el=el, cc=cc: e.activation(out=yb[el][:, cc * 512:(cc + 1) * 512], in_=dp[:],
                                                                  func=AF.Copy), R=[dp], W=[yb[el]])
        for T in range(NT2):
            pt_ = part[0]
            cw["pt"] += 1
            for cc in range(4):
                for el in range(8):
                    S.mm(cps[:], ohwT[el][:, T, :], yb[el][:, cc * 512:(cc + 1) * 512], start=(el == 0),
                         stop=(el == 7), R=[ohwT[el], yb[el]], W=[cps])
                S.act(lambda e, pt_=pt_, cc=cc: e.activation(out=pt_[:, cc * 512:(cc + 1) * 512], in_=cps[:],
                                                             func=AF.Copy), R=[cps], W=[pt_])
            S.dma("sp", D["parts"][G, T * 128:(T + 1) * 128, :], pt_[:], R=[pt_])
    S.flush()
    S.begin()
    gfb = load("gfb", [128, 2048], F32, D["gfb"], "sp")
    eps = S.sb("eps", [128, 1], F32)
    S.dve(lambda e: e.memset(eps[:], EPS), W=[eps])
    xa = S.sbn("xa", 2, [128, 2048], F32)
    pa = S.sbn("pa", 2, [128, 8, 2048], BF16)
    jk = S.sb("jk", [128, 2048], F32)
    ob = S.sbn("ob", 2, [128, 2048], F32)
    ss = S.sbn("ss", 2, [128, 1], F32)
    for T in range(NT2):
        r0 = T * 128
        xa_, pa_, ob_, ss_ = xa[T % 2], pa[T % 2], ob[T % 2], ss[T % 2]
        S.dma("sp", xa_[:], D["x1d"][r0:r0 + 128, :], W=[xa_])
        S.dma("sp", pa_[:], D["parts"][:, r0:r0 + 128, :].rearrange("g t c -> t g c"), W=[pa_])
        for G in range(8):
            eng = S.dve if G % 2 == 0 else S.pool
            eng(lambda e, G=G, xa_=xa_, pa_=pa_: e.tensor_tensor(out=xa_[:], in0=xa_[:], in1=pa_[:, G, :],
                                                                  op=ALU.add), R=[xa_, pa_], W=[xa_])
        S.act(lambda e, xa_=xa_, ss_=ss_: e.activation(out=jk[:], in_=xa_[:], func=AF.Square, accum_out=ss_[:]),
              R=[xa_], W=[jk, ss_])
        S.act(lambda e, ss_=ss_: e.activation(out=ss_[:], in_=ss_[:], func=AF.Sqrt, scale=1.0 / 2048, bias=eps[:]),
              R=[ss_, eps], W=[ss_])
        S.dve(lambda e, ss_=ss_: e.reciprocal(out=ss_[:], in_=ss_[:]), R=[ss_], W=[ss_])
        S.dve(lambda e, xa_=xa_, ss_=ss_, ob_=ob_: e.scalar_tensor_tensor(
            out=ob_[:], in0=xa_[:], scalar=ss_[:, 0:1], in1=gfb[:], op0=ALU.mult, op1=ALU.mult),
            R=[xa_, ss_, gfb], W=[ob_])
        S.dma("sp", D["out"][r0:r0 + 128, :], ob_[:], R=[ob_])


def _build_l1():
    nc = bass.Bass("TRN2", target_bir_lowering=False)
    D = declare_p1a(nc)
    Db = declare_p1b_shared(nc, D)
    S = Sched(nc)
    p1a(S, D)
    p1b(S, Db)
    S.flush(final=True)
    return nc


def declare_p1b_shared(nc, Da):
    D = {"x": Da["x"], "g1": Da["g1"], "ident": Da["ident"]}
    D["wB"] = _din(nc, "wB", [2048, NCOLB])
    D["tab"] = _din(nc, "tab", [32, 8])
    D["tabm"] = _din(nc, "tabm", [32, 33])
    D["ohb"] = _din(nc, "ohb", [33, 4, 64, 128])
    D["ohc"] = _din(nc, "ohc", [33, 12, 64])
    D["wmask"] = _din(nc, "wmask", [128, 2, 256])
    D["ovl"] = _din(nc, "ovl", [128, 2, 64])
    D["fb"] = _din(nc, "fb", [64, 64, 64])
    D["irep"] = _din(nc, "irep", [64, 256])
    for kv in "kv":
        D["w1" + kv] = _din(nc, "w1" + kv, [2048, 256])
        D["w2" + kv] = _din(nc, "w2" + kv, [256, 64])
        D["pe" + kv] = _din(nc, "pe" + kv, [128, 16])
    D["mixB"] = _dout(nc, "mixB", [4096, 512], BF16)
    return D


def _build_l2():
    nc = bass.Bass("TRN2", target_bir_lowering=False)
    D = declare_p23(nc)
    S = Sched(nc)
    p23(S, D)
    S.flush(final=True)
    return nc


def kernel(x, norm1_g, w_in, hg_lb_logits, hg_norm_g, cmp_pe_k, cmp_w1_k, cmp_w2_k, cmp_pe_v, cmp_w1_v,
           cmp_w2_v, rel_bias, w_out, norm2_g, w_router_group, b_router_group, w_router_expert,
           b_router_expert, w_expert_gate, w_expert_up, w_expert_down, final_norm_g):
    A = lambda a: np.asarray(a, dtype=np.float32)
    x = A(x)
    w_in0 = A(w_in)[0]
    cmp = {"pe_k": A(cmp_pe_k)[0], "w1_k": A(cmp_w1_k)[0], "w2_k": A(cmp_w2_k)[0],
           "pe_v": A(cmp_pe_v)[0], "w1_v": A(cmp_w1_v)[0], "w2_v": A(cmp_w2_v)[0]}
    nc1 = _build_l1()
    maps = []
    shared = {}
    for hh in range(2):
        ma = host_p1a(x[0], A(norm1_g)[0], w_in0, A(hg_lb_logits), A(hg_norm_g)[0], hh)
        mb = host_p1b(x[0], A(norm1_g)[0], w_in0, A(rel_bias), cmp, hh)
        ma.update({k: v for k, v in mb.items() if k not in ("x",)})
        shared[hh] = ma
    for k in range(8):
        b, hh = k // 2, k % 2
        m = dict(shared[hh])
        m["x"] = np.ascontiguousarray(x[b])
        maps.append(m)
    res1 = run_bass_kernel_spmd(nc1, maps, core_ids=list(range(8)))
    r1 = res1.results
    mixes = []
    for b in range(4):
        mixes.append(np.concatenate([np.asarray(r1[2 * b]["mixA"]), np.asarray(r1[2 * b + 1]["mixA"]),
                                     np.asarray(r1[2 * b]["mixB"]), np.asarray(r1[2 * b + 1]["mixB"])], axis=1))
    nc2 = _build_l2()
    base = host_p23(x[0, :NTOK], A(w_out)[0], A(norm2_g)[0], A(final_norm_g), A(w_router_group)[0],
                    A(b_router_group)[0], A(w_router_expert)[0], A(b_router_expert)[0],
                    A(w_expert_gate)[0], A(w_expert_up)[0], A(w_expert_down)[0])
    maps2 = []
    for k in range(8):
        b, half = k // 2, k % 2
        m = dict(base)
        m["xo"] = np.ascontiguousarray(x[b, half * NTOK:(half + 1) * NTOK])
        m["mix"] = np.ascontiguousarray(mixes[b][half * NTOK:(half + 1) * NTOK])
        maps2.append(m)
    res2 = run_bass_kernel_spmd(nc2, maps2, core_ids=list(range(8)))
    out = np.empty((4, 4096, 2048), np.float32)
    for k in range(8):
        b, half = k // 2, k % 2
        out[b, half * NTOK:(half + 1) * NTOK] = np.asarray(res2.results[k]["out"])
    return out
```
